# Optimizing a Trainium2 kernel written in Bass

```python
import jax, jax.numpy as jnp
from jax import lax
import numpy as np

D_MODEL = 1024
BATCH = 2
SEQ = 16384
DEPTH = 1

GRID_W = 64
CTX_LEN = 256
HEAD_DIM = 128
N_Q_HEADS = 4
N_KV_HEADS = 2
Q_PER_KV = N_Q_HEADS // N_KV_HEADS
ATTN_WIDTH = N_Q_HEADS * HEAD_DIM
KV_WIDTH = N_KV_HEADS * HEAD_DIM
AXIS_DIM = HEAD_DIM // 2
ROPE_THETA = 10000.0
Q_BLOCK = 128
CONV_WIDTH = D_MODEL - ATTN_WIDTH
CONV_K = 3
MIX_WIDTH = ATTN_WIDTH + CONV_WIDTH
Q_END = ATTN_WIDTH
K_END = Q_END + KV_WIDTH
KV_END = K_END + KV_WIDTH
CB_END = KV_END + CONV_WIDTH
CC_END = CB_END + CONV_WIDTH
IN_COLS = CC_END + CONV_WIDTH
N_EXPERTS = 32
TOP_K = 4
D_FF = D_MODEL
SWIGLU_LIMIT = 7.0
SWIGLU_ALPHA = 1.702
MOE_BLOCK = 128
N_MOD = 6
NORM_EPS = 1e-6

kernel_name = "hybrid_conv_gqa_moe_prefix_dit_block"


def rmsnorm(x, gain):
    x32 = x.astype(jnp.float32)
    y = x32 * lax.rsqrt(jnp.mean(x32 * x32, axis=-1, keepdims=True) + NORM_EPS)
    return y.astype(x.dtype) * gain


def ada_params(cond, w_ada, b_ada):
    mod = jax.nn.silu(cond) @ w_ada + b_ada
    return jnp.split(mod[..., None, :], N_MOD, axis=-1)


def modulate(x, gain, shift, scale):
    return rmsnorm(x, gain) * (1.0 + scale) + shift


def split_in(p):
    return jnp.split(p, [Q_END, K_END, KV_END, CB_END, CC_END], axis=-1)


def to_heads(t, n_heads, gain=None):
    b, n, _ = t.shape
    t = t.reshape(b, n, n_heads, HEAD_DIM)
    return t if gain is None else rmsnorm(t, gain)


def axial_rope_tables(n_tokens):
    rows = n_tokens // GRID_W
    row = jnp.broadcast_to(jnp.arange(rows, dtype=jnp.float32)[:, None], (rows, GRID_W)).reshape(-1)
    col = jnp.broadcast_to(jnp.arange(GRID_W, dtype=jnp.float32)[None, :], (rows, GRID_W)).reshape(-1)
    inv_freq = ROPE_THETA ** (-jnp.arange(0, AXIS_DIM, 2, dtype=jnp.float32) / AXIS_DIM)
    ang = jnp.stack([row[:, None] * inv_freq, col[:, None] * inv_freq], axis=1)
    return jnp.cos(ang), jnp.sin(ang)


def apply_axial_rope(t, cos, sin):
    b, n, h, _ = t.shape
    tr = t.astype(jnp.float32).reshape(b, n, h, 2, 2, AXIS_DIM // 2)
    t1, t2 = tr[..., 0, :], tr[..., 1, :]
    cs, sn = cos[None, :, None], sin[None, :, None]
    out = jnp.stack([t1 * cs - t2 * sn, t2 * cs + t1 * sn], axis=-2)
    return out.reshape(b, n, h, HEAD_DIM).astype(t.dtype)


def dense_attention(q, k, v):
    b, nq, _, _ = q.shape
    qg = q.reshape(b, nq, N_KV_HEADS, Q_PER_KV, HEAD_DIM)
    s = jnp.einsum('bqkgd,bskd->bkgqs', qg, k, preferred_element_type=jnp.float32) * (HEAD_DIM ** -0.5)
    p = jax.nn.softmax(s, axis=-1).astype(v.dtype)
    o = jnp.einsum('bkgqs,bskd->bqkgd', p, v)
    return o.reshape(b, nq, ATTN_WIDTH)


def blocked_attention(q, k, v):
    b, n, _, _ = q.shape
    nblk = n // Q_BLOCK
    qb = q.reshape(b, nblk, Q_BLOCK, N_Q_HEADS, HEAD_DIM).transpose(1, 0, 2, 3, 4)
    o = lax.map(lambda qblk: dense_attention(qblk, k, v), qb)
    return o.transpose(1, 0, 2, 3).reshape(b, n, ATTN_WIDTH)


def short_conv_mixer(cb, cc, cx, conv_w):
    u = cc * cx
    up = jnp.pad(u, ((0, 0), (1, 1), (0, 0)))
    y = up[:, :-2] * conv_w[0] + up[:, 1:-1] * conv_w[1] + up[:, 2:] * conv_w[2]
    return cb * y


def clamped_swiglu(gu):
    glu, lin = jnp.split(gu, 2, axis=-1)
    glu = jnp.minimum(glu, SWIGLU_LIMIT)
    lin = jnp.clip(lin, -SWIGLU_LIMIT, SWIGLU_LIMIT)
    return glu * jax.nn.sigmoid(SWIGLU_ALPHA * glu) * (lin + 1.0)


def moe_ffn(h, w_router, b_router, w_gate_up, b_gate_up, w_down, b_down):
    n_tok, d = h.shape
    logits = jnp.dot(h, w_router, preferred_element_type=jnp.float32) + b_router.astype(jnp.float32)
    top_logit, top_idx = lax.top_k(logits, TOP_K)
    gate = jax.nn.softmax(top_logit, axis=-1)
    n_pair = n_tok * TOP_K
    n_blocks = -(-(n_pair + N_EXPERTS * (MOE_BLOCK - 1)) // MOE_BLOCK)
    n_slots = n_blocks * MOE_BLOCK
    flat_e = top_idx.reshape(-1).astype(jnp.int32)
    flat_tok = jnp.repeat(jnp.arange(n_tok, dtype=jnp.int32), TOP_K, total_repeat_length=n_pair)
    flat_gate = gate.reshape(-1)
    order = jnp.argsort(flat_e, stable=True)
    e_sorted = flat_e[order]
    counts = jnp.zeros((N_EXPERTS,), jnp.int32).at[flat_e].add(1)
    padded = (counts + MOE_BLOCK - 1) // MOE_BLOCK * MOE_BLOCK
    start = jnp.cumsum(counts) - counts
    pad_end = jnp.cumsum(padded)
    pad_start = pad_end - padded
    slot = pad_start[e_sorted] + jnp.arange(n_pair, dtype=jnp.int32) - start[e_sorted]
    slot_tok = jnp.full((n_slots,), n_tok, jnp.int32).at[slot].set(flat_tok[order])
    slot_gate = jnp.zeros((n_slots,), jnp.float32).at[slot].set(flat_gate[order])
    block_start = jnp.arange(n_blocks, dtype=jnp.int32) * MOE_BLOCK
    block_expert = jnp.minimum(jnp.searchsorted(pad_end, block_start, side='right'), N_EXPERTS - 1)
    h_pad = jnp.concatenate([h, jnp.zeros((1, d), h.dtype)], axis=0)

    def expert_block(args):
        tok, g, e = args
        xb = h_pad[tok]
        gu = xb @ w_gate_up[e] + b_gate_up[e]
        y = clamped_swiglu(gu) @ w_down[e] + b_down[e]
        return y * g[:, None].astype(y.dtype)

    y = lax.map(expert_block, (slot_tok.reshape(n_blocks, MOE_BLOCK),
                               slot_gate.reshape(n_blocks, MOE_BLOCK), block_expert))
    out = jnp.zeros((n_tok + 1, d), y.dtype).at[slot_tok].add(y.reshape(n_slots, d))
    return out[:n_tok]


def setup_inputs(seed: int = 0) -> dict:
    key = jax.random.key(seed)
    ks = jax.random.split(key, 20)
    f32 = jnp.float32
    nrm = lambda k, shape, s: jax.random.normal(k, shape, f32) * s
    return {
        "x": nrm(ks[0], (BATCH, SEQ, D_MODEL), 1.0),
        "c": nrm(ks[1], (BATCH, D_MODEL), 1.0),
        "ctx": nrm(ks[2], (BATCH, CTX_LEN, D_MODEL), 1.0),
        "c_ctx": nrm(ks[3], (D_MODEL,), 1.0),
        "w_ada": nrm(ks[4], (DEPTH, D_MODEL, N_MOD * D_MODEL), 0.2 * D_MODEL ** -0.5),
        "b_ada": nrm(ks[5], (DEPTH, N_MOD * D_MODEL), 0.01),
        "g_norm1": 1.0 + nrm(ks[6], (DEPTH, D_MODEL), 0.01),
        "g_norm2": 1.0 + nrm(ks[7], (DEPTH, D_MODEL), 0.01),
        "w_in": nrm(ks[8], (DEPTH, D_MODEL, IN_COLS), D_MODEL ** -0.5),
        "g_q": 1.0 + nrm(ks[9], (DEPTH, HEAD_DIM), 0.01),
        "g_k": 1.0 + nrm(ks[10], (DEPTH, HEAD_DIM), 0.01),
        "w_conv": nrm(ks[11], (DEPTH, CONV_K, CONV_WIDTH), CONV_K ** -0.5),
        "w_out": nrm(ks[12], (DEPTH, MIX_WIDTH, D_MODEL), MIX_WIDTH ** -0.5),
        "w_router": nrm(ks[13], (DEPTH, D_MODEL, N_EXPERTS), D_MODEL ** -0.5),
        "b_router": nrm(ks[14], (DEPTH, N_EXPERTS), 0.01),
        "w_gate_up": nrm(ks[15], (DEPTH, N_EXPERTS, D_MODEL, 2 * D_FF), D_MODEL ** -0.5),
        "b_gate_up": nrm(ks[16], (DEPTH, N_EXPERTS, 2 * D_FF), 0.01),
        "w_down": nrm(ks[17], (DEPTH, N_EXPERTS, D_FF, D_MODEL), D_FF ** -0.5),
        "b_down": nrm(ks[18], (DEPTH, N_EXPERTS, D_MODEL), 0.01),
        "g_final": 1.0 + nrm(ks[19], (D_MODEL,), 0.01),
    }


def reference(x, c, ctx, c_ctx, w_ada, b_ada, g_norm1, g_norm2, w_in, g_q, g_k, w_conv, w_out,
              w_router, b_router, w_gate_up, b_gate_up, w_down, b_down, g_final):
    b, n, d = x.shape
    cos, sin = axial_rope_tables(n)
    for layer in range(DEPTH):
        last = layer == DEPTH - 1
        sh1, sc1, gt1, sh2, sc2, gt2 = ada_params(c, w_ada[layer], b_ada[layer])
        csh1, csc1, cgt1, csh2, csc2, cgt2 = ada_params(c_ctx, w_ada[layer], b_ada[layer])
        w_in_l = w_in[layer]

        hc = modulate(ctx, g_norm1[layer], csh1, csc1)
        if last:
            kc, vc = jnp.split(hc @ w_in_l[:, Q_END:KV_END], 2, axis=-1)
        else:
            qc, kc, vc, cbc, ccc, cxc = split_in(hc @ w_in_l)
        kc = to_heads(kc, N_KV_HEADS, g_k[layer])
        vc = to_heads(vc, N_KV_HEADS)
        if not last:
            qc = to_heads(qc, N_Q_HEADS, g_q[layer])
            mix_c = jnp.concatenate([dense_attention(qc, kc, vc),
                                     short_conv_mixer(cbc, ccc, cxc, w_conv[layer])], axis=-1)
            ctx_next = ctx + cgt1 * (mix_c @ w_out[layer])
            hc2 = modulate(ctx_next, g_norm2[layer], csh2, csc2).reshape(-1, d)
            ctx_next = ctx_next + cgt2 * moe_ffn(hc2, w_router[layer], b_router[layer], w_gate_up[layer],
                                                 b_gate_up[layer], w_down[layer], b_down[layer]).reshape(ctx.shape)

        h = modulate(x, g_norm1[layer], sh1, sc1)
        q, k, v, cb, cc, cx = split_in(h @ w_in_l)
        q = apply_axial_rope(to_heads(q, N_Q_HEADS, g_q[layer]), cos, sin)
        k = apply_axial_rope(to_heads(k, N_KV_HEADS, g_k[layer]), cos, sin)
        v = to_heads(v, N_KV_HEADS)
        k_all = jnp.concatenate([k, kc], axis=1)
        v_all = jnp.concatenate([v, vc], axis=1)
        attn = blocked_attention(q, k_all, v_all)
        conv = short_conv_mixer(cb, cc, cx, w_conv[layer])
        x = x + gt1 * (jnp.concatenate([attn, conv], axis=-1) @ w_out[layer])

        h2 = modulate(x, g_norm2[layer], sh2, sc2).reshape(-1, d)
        x = x + gt2 * moe_ffn(h2, w_router[layer], b_router[layer], w_gate_up[layer], b_gate_up[layer],
                              w_down[layer], b_down[layer]).reshape(b, n, d)
        if not last:
            ctx = ctx_next
    return rmsnorm(x, g_final)
```

```python
import math
import contextlib
import numpy as np
import concourse.bass as bass
import concourse.mybir as mybir
from concourse.bass_utils import run_bass_kernel_spmd

F32 = mybir.dt.float32
BF16 = mybir.dt.bfloat16
I32 = mybir.dt.int32
AF = mybir.ActivationFunctionType
ALU = mybir.AluOpType
AX = mybir.AxisListType

ENGS = ("pe", "act", "dve", "pool", "sp")
D = 1024
KD = 8
NE = 32
EPS = 1e-6


class _Op:
    __slots__ = ("eng", "fn", "reads", "writes", "dma", "deps", "signal", "sig_idx",
                 "dsem", "dval", "prev_same_slot", "idx")


class _Rec:
    def __init__(self):
        self.call = None

    def __getattr__(self, name):
        def f(*a, **k):
            self.call = (name, a, k)
            return self
        return f


class _RegRef:
    def __init__(self, eng, value):
        self.eng, self.value = eng, value


class Prog:
    def reg_const(self, eng, value):
        r = _RegRef(eng, value)
        self.reg_consts.append(r)
        return r

    def __init__(self, nc, n_dma_sems=24):
        self.reg_consts = []
        self.ever_w = set()
        self.never_written = set()
        self.nc = nc
        self.ops = []
        self.last_w = {}
        self.readers = {}
        self.n_dma_sems = n_dma_sems
        self.last_on_eng = {}
        self.dmas_since_barrier = []
        self.barrier_op = None

    def _add(self, eng, fn, reads, writes, dma, extra_deps=()):
        o = _Op()
        rec = _Rec()
        fn(rec)
        assert rec.call is not None
        o.eng, o.fn, o.dma = eng, rec.call, dma
        o.reads, o.writes = frozenset(reads), frozenset(writes)
        o.signal = False
        o.sig_idx = 0
        o.idx = len(self.ops)
        deps = set(extra_deps)
        if self.barrier_op is not None:
            deps.add(self.barrier_op)
        for k in o.reads:
            w = self.last_w.get(k)
            if w is not None:
                deps.add(w)
            elif k not in self.ever_w:
                self.never_written.add(k)
        self.ever_w.update(o.writes)
        for k in o.writes:
            w = self.last_w.get(k)
            if w is not None:
                deps.add(w)
            for r in self.readers.get(k, ()):
                deps.add(r)
        deps.discard(o.idx)
        o.deps = deps
        for k in o.reads:
            if k not in o.writes:
                self.readers.setdefault(k, []).append(o.idx)
        for k in o.writes:
            self.last_w[k] = o.idx
            self.readers[k] = []
        self.ops.append(o)
        if dma:
            self.dmas_since_barrier.append(o.idx)
        else:
            self.last_on_eng[eng] = o.idx
        return o

    def op(self, eng, fn, reads=(), writes=()):
        return self._add(eng, fn, reads, writes, False)

    def dma(self, eng, fn, reads=(), writes=()):
        return self._add(eng, fn, reads, writes, True)

    def barrier(self):
        extra = set(self.last_on_eng.values()) | set(self.dmas_since_barrier)
        o = self._add("sp", lambda e: e.nop(), (), (), False, extra_deps=extra)
        self.barrier_op = o.idx
        self.dmas_since_barrier = []
        self.last_w = {}
        self.readers = {}
        return o

    def _needs_sync(self, p, o):
        if p.eng != o.eng or o.dma:
            return True
        if p.eng == "pe":
            return False
        return bool(p.writes & o.reads)

    def emit(self):
        nc = self.nc
        ops = self.ops
        for o in ops:
            for d in o.deps:
                p = ops[d]
                if p.dma:
                    continue
                if self._needs_sync(p, o):
                    p.signal = True
        cnt = {e: 0 for e in ENGS}
        for o in ops:
            if not o.dma and o.signal:
                cnt[o.eng] += 1
                o.sig_idx = cnt[o.eng]
        dma_by_eng = {e: [] for e in ENGS}
        for o in ops:
            if o.dma:
                lst = dma_by_eng[o.eng]
                j = len(lst)
                o.dsem = (o.eng, j % self.n_dma_sems)
                o.dval = 16 * (j // self.n_dma_sems + 1)
                o.prev_same_slot = lst[j - self.n_dma_sems] if j >= self.n_dma_sems else None
                lst.append(o)
        per_eng = {e: [o for o in ops if o.eng == e] for e in ENGS}
        self.stats = {e: len(per_eng[e]) for e in ENGS}
        with contextlib.ExitStack() as st:
            csem = {e: st.enter_context(nc.semaphore(f"c_{e}")) for e in ENGS}
            dsem = {}
            for e in ENGS:
                for s in range(min(self.n_dma_sems, len(dma_by_eng[e]))):
                    dsem[(e, s)] = st.enter_context(nc.semaphore(f"d_{e}{s}"))
            block = st.enter_context(nc.Block())

            def run(e, engobj):
                seen_c = {x: 0 for x in ENGS}
                seen_d = {}
                regmap = {}
                for rr in self.reg_consts:
                    if rr.eng == e:
                        h_ = engobj.alloc_register(f"rc{len(regmap)}")
                        engobj.reg_mov(h_, rr.value)
                        regmap[id(rr)] = h_
                for o in per_eng[e]:
                    need_c = {}
                    need_d = {}
                    if o.dma and o.prev_same_slot is not None:
                        p = o.prev_same_slot
                        need_d[p.dsem] = p.dval
                    for d in o.deps:
                        p = ops[d]
                        if p.dma:
                            if p.dval > need_d.get(p.dsem, 0):
                                need_d[p.dsem] = p.dval
                        elif self._needs_sync(p, o):
                            if p.sig_idx > need_c.get(p.eng, 0):
                                need_c[p.eng] = p.sig_idx
                    for pe_, v in need_c.items():
                        if v > seen_c[pe_]:
                            engobj.wait_ge(csem[pe_], v)
                            seen_c[pe_] = v
                    for k, v in need_d.items():
                        if v > seen_d.get(k, 0):
                            engobj.wait_ge(dsem[k], v)
                            seen_d[k] = v
                    name_, a_, k_ = o.fn
                    k_ = {kk: (regmap[id(vv)] if isinstance(vv, _RegRef) else vv) for kk, vv in k_.items()}
                    try:
                        ins = getattr(engobj, name_)(*a_, **k_)
                    except Exception:
                        print("EMIT FAILURE", e, name_, {kk: str(vv)[:300] for kk, vv in k_.items()}, [str(x)[:300] for x in a_])
                        raise
                    if o.dma:
                        ins.then_inc(dsem[o.dsem], 16)
                    elif o.signal:
                        ins.then_inc(csem[o.eng], 1)
                for o in dma_by_eng[e][-self.n_dma_sems:]:
                    if o.dval > seen_d.get(o.dsem, 0):
                        engobj.wait_ge(dsem[o.dsem], o.dval)
                        seen_d[o.dsem] = o.dval

            if per_eng["sp"]:
                @block.sync
                def _(eng):
                    run("sp", eng)
            if per_eng["act"]:
                @block.scalar
                def _(eng):
                    run("act", eng)
            if per_eng["dve"]:
                @block.vector
                def _(eng):
                    run("dve", eng)
            if per_eng["pool"]:
                @block.gpsimd
                def _(eng):
                    run("pool", eng)
            if per_eng["pe"]:
                @block.tensor
                def _(eng):
                    run("pe", eng)


class Arena:
    def __init__(self, tensor, n):
        self.t = tensor
        self.n = n
        self.off = 0
        self.peak = 0

    def mark(self):
        return self.off

    def reset(self, m):
        self.off = m

    def alloc(self, shape, dt):
        nel = int(np.prod(shape[1:]))
        w = 1 if dt == BF16 else 2
        if self.off % 2:
            self.off += 1
        a = self.off
        self.off += nel * w
        assert self.off <= self.n, f"arena overflow {self.off} > {self.n}"
        self.peak = max(self.peak, self.off)
        v = self.t[0:shape[0], a:a + nel * w]
        if dt != BF16:
            v = v.bitcast(dt)
        if len(shape) == 3:
            v = v.rearrange("p (a b) -> p a b", a=shape[1])
        elif len(shape) == 4:
            v = v.rearrange("p (a b c) -> p a b c", a=shape[1], b=shape[2])
        return v


def build(S, C, dbg=False, stop=99):
    NT = S // 128
    TQ = S // 4
    NQ = TQ // 128
    NG = NQ // 4
    NK = NT + 2
    SK = S + 256
    NROW = S // 64
    SN = min(512, C)
    NSB = C // 128
    BIG = float(1 << 20)
    assert NQ % 4 == 0 and C % 128 == 0 and C % SN == 0

    nc = bass.Bass("TRN2", target_bir_lowering=False)

    def din(name, shape, dt=F32):
        return nc.dram_tensor(name, list(shape), dt, kind="ExternalInput").ap()

    def dscr(name, shape, dt):
        kind = "ExternalOutput" if dbg else "Internal"
        return nc.dram_tensor(name, list(shape), dt, kind=kind).ap()

    xr = din("xr", [S, D])
    xh = din("xh", [2, D])
    hm = din("hm", [128, 2])
    r0 = din("r0", [128, 1])
    cvec = din("cvec", [2, D])
    ctxb = din("ctxb", [256, D])
    w_ada = din("w_ada", [D, 6 * D])
    b_ada = din("b_ada", [1, 6 * D])
    g_norm1 = din("g_norm1", [D])
    g_norm2 = din("g_norm2", [D])
    w_in = din("w_in", [D, 2560])
    g_q = din("g_q", [128])
    g_k = din("g_k", [128])
    w_conv = din("w_conv", [3, 512])
    w_out = din("w_out", [D, D])
    w_router = din("w_router", [D, NE])
    b_router = din("b_router", [NE])
    moe_in = stop > 4
    w_gate_up = din("w_gate_up", [NE, D, 2 * D] if moe_in else [1, 1, 2])
    b_gate_up = din("b_gate_up", [NE, 2 * D])
    w_down = din("w_down", [NE, D, D] if moe_in else [1, 1, 2])
    b_down = din("b_down", [NE, D])
    g_final = din("g_final", [D])
    yout = nc.dram_tensor("yout", [TQ, D], F32, kind="ExternalOutput").ap()

    modrows = dscr("modrows", [2, 6, D], F32)
    Ks = dscr("Ks", [2, 128, SK], BF16)
    Vs = dscr("Vs", [2, SK, 128], BF16)
    Qs = dscr("Qs", [4, 128, TQ], BF16)
    Cs = dscr("Cs", [4, 128, TQ], BF16)
    As = dscr("As", [4, 128, TQ], BF16)
    X1 = dscr("X1", [TQ, D], F32)
    XB = dscr("XB", [NE * C, D], BF16)
    Y = dscr("Y", [NE * C, D], F32)
    if dbg:
        LG = dscr("LG", [TQ, NE], F32)
        DST = dscr("DST", [TQ, 4], I32)
        GKD = dscr("GKD", [TQ, 4], F32)
        H2 = dscr("H2", [TQ, D], F32)

    P = Prog(nc)
    BCREG = P.reg_const("pool", NE * C - 1)
    st = contextlib.ExitStack()
    with st:
        def sb(name, shape, dt):
            return st.enter_context(nc.sbuf_tensor(name, list(shape), dt))

        identf = sb("identf", [128, 128], F32)
        identb = sb("identb", [128, 128], BF16)
        onesb = sb("onesb", [128, 128], BF16)
        utri = sb("utri", [128, 128], BF16)
        epsb = sb("epsb", [128, 1], F32)
        negpi = sb("negpi", [128, 1], F32)
        zerob = sb("zerob", [128, 1], F32)
        hmt = sb("hmt", [128, 2], F32)
        r0t = sb("r0t", [128, 1], F32)
        ARENA_N = 104400
        arena_t = sb("arena", [128, ARENA_N], BF16)
        A = Arena(arena_t, ARENA_N)
        pb = [st.enter_context(nc.psum_tensor(f"pb{i}", [128, 512], F32)) for i in range(8)]

        def pbf(i):
            return pb[i][:, :]

        def pbb(i):
            return pb[i][:, :].bitcast(BF16)

        P.op("pool", lambda e: e.memset(identf[:], 0.0), writes=["identf"])
        P.op("pool", lambda e: e.affine_select(out=identf[:], in_=identf[:], pattern=[[-1, 128]],
                                               compare_op=ALU.not_equal, fill=1.0, base=0, channel_multiplier=1),
             reads=["identf"], writes=["identf"])
        P.op("pool", lambda e: e.tensor_copy(out=identb[:], in_=identf[:]), reads=["identf"], writes=["identb"])
        P.op("pool", lambda e: e.memset(onesb[:], 1.0), writes=["onesb"])
        P.op("pool", lambda e: e.memset(utri[:], 1.0), writes=["utri"])
        P.op("pool", lambda e: e.affine_select(out=utri[:], in_=utri[:], pattern=[[1, 128]],
                                               compare_op=ALU.is_gt, fill=0.0, base=0, channel_multiplier=-1),
             reads=["utri"], writes=["utri"])
        P.op("dve", lambda e: e.memset(epsb[:], EPS), writes=["epsb"])
        P.op("dve", lambda e: e.memset(negpi[:], -math.pi), writes=["negpi"])
        P.op("dve", lambda e: e.memset(zerob[:], 0.0), writes=["zerob"])
        P.dma("sp", lambda e: e.dma_start(out=hmt[:], in_=hm), writes=["hmt"])
        P.dma("sp", lambda e: e.dma_start(out=r0t[:], in_=r0), writes=["r0t"])

        m0 = A.mark()
        csb = A.alloc([128, 2, KD], F32)
        scb = A.alloc([128, KD, 2], BF16)
        bada = A.alloc([1, 6 * D], BF16)
        mods = A.alloc([2, 6 * D], F32)
        gcomp = A.alloc([2, 2, D], F32)
        g12 = A.alloc([2, 2, D], F32)
        wa = [A.alloc([128, KD, 512], BF16) for _ in range(2)]
        for j in range(2):
            P.dma("sp", lambda e, j=j: e.dma_start(out=csb[:, j, :], in_=cvec[j, :].rearrange("(k p) -> p k", p=128),
                                                   allow_slow_non_contiguous=True), writes=[f"csb{j}"])
        P.op("act", lambda e: e.activation(out=scb, in_=csb.rearrange("p j k -> p k j"), func=AF.Silu),
             reads=["csb0", "csb1"], writes=["scb"])
        P.dma("pool", lambda e: e.dma_start(out=bada, in_=b_ada), writes=["bada"])
        P.dma("sp", lambda e: e.dma_start(out=g12[:, 0, :], in_=g_norm1.partition_broadcast(2)), writes=["g12a"])
        P.dma("sp", lambda e: e.dma_start(out=g12[:, 1, :], in_=g_norm2.partition_broadcast(2)), writes=["g12b"])
        for n in range(12):
            w = wa[n % 2]
            P.dma("pool", lambda e, w=w, n=n: e.dma_start(
                out=w, in_=w_ada[:, n * 512:(n + 1) * 512].rearrange("(k p) n -> p k n", p=128)),
                writes=[f"wa{n % 2}"])
            pm = pbf(n % 2)
            for k in range(KD):
                P.op("pe", lambda e, w=w, k=k, pm=pm: e.matmul(pm[0:2, :], lhsT=scb[:, k, :], rhs=w[:, k, :],
                                                                start=(k == 0), stop=False),
                     reads=["scb", f"wa{n % 2}"], writes=[f"pb{n % 2}"])
            P.op("pe", lambda e, n=n, pm=pm: e.matmul(pm[0:2, :], lhsT=onesb[0:1, 0:2],
                                                      rhs=bada[0:1, n * 512:(n + 1) * 512], start=False, stop=True),
                 reads=["onesb", "bada"], writes=[f"pb{n % 2}"])
            P.op("act", lambda e, n=n, pm=pm: e.copy(out=mods[0:2, n * 512:(n + 1) * 512], in_=pm[0:2, :]),
                 reads=[f"pb{n % 2}"], writes=["mods"])
        P.op("dve", lambda e: e.scalar_tensor_tensor(out=gcomp[:, 0, :], in0=mods[0:2, D:2 * D], scalar=1.0,
                                                     in1=g12[:, 0, :], op0=ALU.add, op1=ALU.mult),
             reads=["mods", "g12a"], writes=["gcomp"])
        P.op("dve", lambda e: e.scalar_tensor_tensor(out=gcomp[:, 1, :], in0=mods[0:2, 4 * D:5 * D], scalar=1.0,
                                                     in1=g12[:, 1, :], op0=ALU.add, op1=ALU.mult),
             reads=["mods", "g12b", "gcomp"], writes=["gcomp"])
        for slot, src in ((0, 0), (2, 2), (3, 3), (5, 5)):
            P.dma("sp", lambda e, slot=slot, src=src: e.dma_start(out=modrows[:, slot, :],
                                                                  in_=mods[0:2, src * D:(src + 1) * D]),
                  reads=["mods"], writes=["modrows"])
        P.dma("sp", lambda e: e.dma_start(out=modrows[:, 1, :], in_=gcomp[:, 0, :]), reads=["gcomp"], writes=["modrows"])
        P.dma("sp", lambda e: e.dma_start(out=modrows[:, 4, :], in_=gcomp[:, 1, :]), reads=["gcomp"], writes=["modrows"])
        P.barrier()
        A.reset(m0)
        if stop <= 0:
            P.emit()
            return nc, P, A

        def load_mod(dst, j, slot, key):
            P.dma("sp", lambda e: e.dma_start(out=dst, in_=modrows[j, slot, :].partition_broadcast(128)),
                  writes=[key])

        Wi = A.alloc([128, KD, 2560], BF16)
        MG = A.alloc([128, D], F32)
        MS = A.alloc([128, D], F32)
        GQK = A.alloc([128, 6, 128], F32)
        ROWC = A.alloc([128, NT, 32], F32)
        ROWS = A.alloc([128, NT, 32], F32)
        COLC = A.alloc([128, 32], F32)
        COLS = A.alloc([128, 32], F32)
        invf = A.alloc([128, 32], F32)
        wcv = A.alloc([128, 3, 4], F32)
        xts = [A.alloc([128, D], F32) for _ in range(2)]
        tmpf = A.alloc([128, D], F32)
        hbs = [A.alloc([128, D], BF16) for _ in range(2)]
        hTs = [A.alloc([128, KD, 512], BF16) for _ in range(2)]
        qk6s = [A.alloc([128, 6, 128], F32) for _ in range(2)]
        ro1s = [A.alloc([128, 6, 128], F32) for _ in range(2)]
        ro2s = [A.alloc([128, 6, 128], F32) for _ in range(2)]
        qkbs = [A.alloc([128, 6, 128], BF16) for _ in range(2)]
        kTst = [A.alloc([128, 6, 512], BF16) for _ in range(2)]
        vst = [A.alloc([128, 4, 256], BF16) for _ in range(2)]
        ccs = [A.alloc([128, 512], F32) for _ in range(1)] * 2
        cbT = [A.alloc([128, 4, 512], BF16) for _ in range(2)]
        uT = A.alloc([128, 4, TQ + 2], BF16)
        cvt = A.alloc([128, 512], F32)
        cvo = [A.alloc([128, 4, 512], BF16) for _ in range(1)] * 2
        small = A.alloc([128, 64], F32)
        ss6s = [small[:, 8:14], small[:, 32:38]]
        rs6s = [small[:, 16:22], small[:, 40:46]]
        rowf = A.alloc([128, NT], F32)
        roww = A.alloc([128, NT], F32)
        colf = A.alloc([128, 1], F32)
        phf = A.alloc([128, 1], F32)
        COLA = A.alloc([128, 32], F32)

        for c5 in range(5):
            P.dma("pool", lambda e, c5=c5: e.dma_start(
                out=Wi[:, :, c5 * 512:(c5 + 1) * 512],
                in_=w_in[:, c5 * 512:(c5 + 1) * 512].rearrange("(k p) n -> p k n", p=128)), writes=[f"Wi{c5}"])
        WI_ALL = [f"Wi{c5}" for c5 in range(5)]
        for hh in range(4):
            P.dma("sp", lambda e, hh=hh: e.dma_start(out=GQK[:, hh, :], in_=g_q.partition_broadcast(128)),
                  writes=[f"gqk{hh}"])
        for hh in range(4, 6):
            P.dma("sp", lambda e, hh=hh: e.dma_start(out=GQK[:, hh, :], in_=g_k.partition_broadcast(128)),
                  writes=[f"gqk{hh}"])
        P.op("dve", lambda e: e.tensor_scalar(out=GQK[:, 0:4, :], in0=GQK[:, 0:4, :], scalar1=128.0 ** -0.5,
                                              scalar2=None, op0=ALU.mult),
             reads=[f"gqk{h}" for h in range(4)], writes=["GQKq"])
        GQK_KEYS = ["GQKq", "gqk4", "gqk5"]
        for t_ in range(3):
            P.dma("sp", lambda e, t_=t_: e.dma_start(out=wcv[:, t_, :], in_=w_conv[t_, :].rearrange("(c p) -> p c", p=128),
                                                     allow_slow_non_contiguous=True), writes=[f"wcv{t_}"])

        P.op("pool", lambda e: e.iota(colf, pattern=[[0, 1]], base=0, channel_multiplier=1,
                                      allow_small_or_imprecise_dtypes=True), writes=["colf"])
        P.op("dve", lambda e: e.tensor_scalar(out=phf, in0=colf, scalar1=64.0, scalar2=None, op0=ALU.is_ge),
             reads=["colf"], writes=["phf"])
        P.op("dve", lambda e: e.scalar_tensor_tensor(out=colf, in0=phf, scalar=-64.0, in1=colf, op0=ALU.mult, op1=ALU.add),
             reads=["phf", "colf"], writes=["colf"])
        P.op("dve", lambda e: e.tensor_tensor(out=phf, in0=phf, in1=r0t[:, 0:1], op=ALU.add), reads=["phf", "r0t"], writes=["phf"])
        P.op("pool", lambda e: e.iota(rowf, pattern=[[2, NT]], base=0, channel_multiplier=0,
                                      allow_small_or_imprecise_dtypes=True), writes=["rowf"])
        P.op("dve", lambda e: e.tensor_scalar(out=rowf, in0=rowf, scalar1=phf[:, 0:1], scalar2=None, op0=ALU.add),
             reads=["rowf", "phf"], writes=["rowf"])
        P.op("dve", lambda e: e.tensor_scalar(out=roww, in0=rowf, scalar1=float(NROW), scalar2=float(NROW),
                                              op0=ALU.is_ge, op1=ALU.mult), reads=["rowf"], writes=["roww"])
        P.op("dve", lambda e: e.tensor_tensor(out=rowf, in0=rowf, in1=roww, op=ALU.subtract), reads=["rowf", "roww"], writes=["rowf"])
        P.op("pool", lambda e: e.iota(invf, pattern=[[1, 32]], base=0, channel_multiplier=0,
                                      allow_small_or_imprecise_dtypes=True), writes=["invf"])
        P.op("act", lambda e: e.activation(out=invf, in_=invf, func=AF.Exp, scale=-math.log(10000.0) * 2.0 / 64.0),
             reads=["invf"], writes=["invf"])
        TWO_PI = 2 * math.pi
        tA = ro1s[0].rearrange("p a b -> p (a b)")[:, 0:512]
        tB = qk6s[0].rearrange("p a b -> p (a b)")[:, 0:512]
        tC = tmpf[:, 0:512]
        tI = xts[1][:, 0:512].bitcast(I32)

        def sin_table(dst, ang, add, shp):
            b_, c_, i_ = (tB, tC, tI)
            if len(shp) == 3:
                b_, c_, i_ = (t_[:, 0:shp[1] * shp[2]].rearrange("p (a b) -> p a b", a=shp[1]) for t_ in (tB, tC, tI))
            else:
                b_, c_, i_ = (t_[:, 0:shp[1]] for t_ in (tB, tC, tI))
            P.op("dve", lambda e: e.tensor_scalar(out=b_, in0=ang, scalar1=add, scalar2=1.0 / TWO_PI, op0=ALU.add, op1=ALU.mult),
                 reads=["ro10_0", "ro11_0", "COLA"], writes=["qk6_0"])
            P.op("dve", lambda e: e.tensor_copy(out=i_, in_=b_), reads=["qk6_0"], writes=["xt1"])
            P.op("dve", lambda e: e.tensor_copy(out=b_, in_=i_), reads=["xt1"], writes=["qk6_0"])
            P.op("dve", lambda e: e.tensor_scalar(out=c_, in0=ang, scalar1=add, scalar2=None, op0=ALU.add),
                 reads=["ro10_0", "ro11_0", "COLA"], writes=["tmpf"])
            P.op("dve", lambda e: e.scalar_tensor_tensor(out=c_, in0=b_, scalar=-TWO_PI, in1=c_, op0=ALU.mult, op1=ALU.add),
                 reads=["qk6_0", "tmpf"], writes=["tmpf"])
            P.op("dve", lambda e: e.tensor_scalar(out=c_, in0=c_, scalar1=-math.pi, scalar2=math.pi, op0=ALU.max, op1=ALU.min),
                 reads=["tmpf"], writes=["tmpf"])
            P.op("act", lambda e: e.activation(out=dst, in_=c_, func=AF.Sin), reads=["tmpf"], writes=["ROPE"])

        CH = 16
        for c0 in range(0, NT, CH):
            n_ = min(CH, NT - c0)
            a_ = tA[:, 0:n_ * 32].rearrange("p (a b) -> p a b", a=n_)
            P.op("dve", lambda e: e.tensor_tensor(out=a_, in0=rowf[:, c0:c0 + n_].unsqueeze(2).to_broadcast([128, n_, 32]),
                                                  in1=invf.unsqueeze(1).to_broadcast([128, n_, 32]), op=ALU.mult),
                 reads=["rowf", "invf"], writes=["ro10_0", "ro11_0"])
            sin_table(ROWS[:, c0:c0 + n_, :], a_, 0.0, [128, n_, 32])
            sin_table(ROWC[:, c0:c0 + n_, :], a_, 0.5 * math.pi, [128, n_, 32])
        P.op("dve", lambda e: e.tensor_scalar(out=COLA, in0=invf, scalar1=colf[:, 0:1], scalar2=None, op0=ALU.mult),
             reads=["invf", "colf"], writes=["COLA"])
        sin_table(COLS, COLA, 0.0, [128, 32])
        sin_table(COLC, COLA, 0.5 * math.pi, [128, 32])

        state = {"xi": 0, "hi": 0, "qi": 0}

        def stageA1(src_ap, nrows):
            xi = state["xi"] % 2
            state["xi"] += 1
            xt = xts[xi]
            hb = hbs[xi]
            xk, hk = f"xt{xi}", f"hb{xi}"
            ss1, rs1 = small[:, 2 * xi:2 * xi + 1], small[:, 2 * xi + 1:2 * xi + 2]
            sk_, rk_ = f"ss1_{xi}", f"rs1_{xi}"
            if nrows < 128:
                P.op("pool", lambda e: e.memset(xt, 0.0), writes=[xk])
            P.dma("sp", lambda e: e.dma_start(out=xt[0:nrows, :], in_=src_ap), writes=[xk])
            P.op("act", lambda e: e.activation(out=hb, in_=xt, func=AF.Square, accum_out=ss1),
                 reads=[xk], writes=[hk, sk_])
            P.op("act", lambda e: e.activation(out=rs1, in_=ss1, func=AF.Sqrt, scale=1.0 / D, bias=epsb[:, 0:1]),
                 reads=[sk_, "epsb"], writes=[rk_])
            P.op("dve", lambda e: e.reciprocal(out=rs1, in_=rs1), reads=[rk_], writes=[rk_])
            return (xt, xk, hb, hk, rs1, rk_)

        def stageA2(a1):
            xt, xk, hb, hk, rs1, rk_ = a1
            P.op("dve", lambda e: e.scalar_tensor_tensor(out=tmpf, in0=xt, scalar=rs1, in1=MG, op0=ALU.mult,
                                                         op1=ALU.mult), reads=[xk, rk_, "MG"], writes=["tmpf"])
            P.op("pool", lambda e: e.tensor_tensor(out=hb, in0=tmpf, in1=MS, op=ALU.add),
                 reads=["tmpf", "MS"], writes=[hk])
            return (hb, hk)

        def stageA(src_ap, nrows):
            return stageA2(stageA1(src_ap, nrows))

        def stageB1(a_, hT_dst, hkey):
            hb, hk = a_
            pT = pbb(7).rearrange("p (k n) -> p k n", k=KD)
            for k in range(KD):
                P.op("pe", lambda e, k=k: e.transpose(out=pT[:, k, :], in_=hb[:, k * 128:(k + 1) * 128],
                                                      identity=identb[:]),
                     reads=[hk, "identb"], writes=["pb7"])
            P.op("act", lambda e: e.copy(out=hT_dst, in_=pT), reads=["pb7"], writes=[hkey])

        def norm_tile(src_ap, nrows, hT_dst, hkey):
            stageB1(stageA(src_ap, nrows), hT_dst, hkey)

        def qk_pre(nheads, h0, ps_list):
            hs = slice(h0, h0 + nheads)
            z = state["qi"] % 2
            state["qi"] += 1
            qk6, ro1, ro2, qkb, ss6, rs6 = qk6s[z], ro1s[z], ro2s[z], qkbs[z], ss6s[z], rs6s[z]
            sq6 = ro1
            Z = f"_{z}"
            for (ps_ap, pkey, nh, ho) in ps_list:
                P.op("act", lambda e, ps_ap=ps_ap, nh=nh, ho=ho: e.activation(
                    out=sq6[:, ho:ho + nh, :], in_=ps_ap.rearrange("p (h d) -> p h d", h=nh), func=AF.Square),
                    reads=[pkey], writes=["ro10" + Z, "ro11" + Z])
            P.op("dve", lambda e: e.tensor_reduce(out=ss6[:, hs], in_=sq6[:, hs, :], axis=AX.X, op=ALU.add),
                 reads=["ro10" + Z, "ro11" + Z], writes=["ss6" + Z])
            P.op("act", lambda e: e.activation(out=rs6[:, hs], in_=ss6[:, hs], func=AF.Sqrt, scale=1.0 / 128,
                                               bias=epsb[:, 0:1]), reads=["ss6" + Z, "epsb"], writes=["rs6" + Z])
            P.op("dve", lambda e: e.reciprocal(out=rs6[:, hs], in_=rs6[:, hs]), reads=["rs6" + Z], writes=["rs6" + Z])
            for (ps_ap, pkey, nh, ho) in ps_list:
                P.op("dve", lambda e, ps_ap=ps_ap, nh=nh, ho=ho: e.tensor_tensor(
                    out=qk6[:, ho:ho + nh, :], in0=ps_ap.rearrange("p (h d) -> p h d", h=nh),
                    in1=GQK[:, ho:ho + nh, :], op=ALU.mult), reads=[pkey] + GQK_KEYS, writes=["qk6" + Z])
            return (nheads, h0, z)

        def qk_c1(q_, rope_tile):
            nheads, h0, z = q_
            hs = slice(h0, h0 + nheads)
            qk6, ro1, ro2, qkb, ss6, rs6 = qk6s[z], ro1s[z], ro2s[z], qkbs[z], ss6s[z], rs6s[z]
            Z = f"_{z}"
            pT = pbb(6 if z == 0 else 5).rearrange("p (k n) -> p k n", k=KD)
            pTk = "pb6" if z == 0 else "pb5"
            if rope_tile is None:
                P.op("dve", lambda e: e.tensor_tensor(out=qkb[:, hs, :], in0=qk6[:, hs, :],
                                                      in1=rs6[:, hs].unsqueeze(2).to_broadcast([128, nheads, 128]),
                                                      op=ALU.mult), reads=["qk6" + Z, "rs6" + Z], writes=["qkb" + Z])
            else:
                P.op("dve", lambda e: e.tensor_tensor(out=qk6[:, hs, :], in0=qk6[:, hs, :],
                                                      in1=rs6[:, hs].unsqueeze(2).to_broadcast([128, nheads, 128]),
                                                      op=ALU.mult), reads=["qk6" + Z, "rs6" + Z], writes=["qk6" + Z])
                i = rope_tile
                for a in range(2):
                    Ct = ROWC[:, i, :] if a == 0 else COLC
                    St = ROWS[:, i, :] if a == 0 else COLS
                    ck = "ROPE"
                    sk = "ROPE"

                    def v(t, half=None):
                        w = t[:, hs, a * 64:(a + 1) * 64]
                        if half is None:
                            return w.rearrange("p h (f j) -> p h f j", f=2)
                        return w[:, :, half * 32:(half + 1) * 32]
                    eng = "dve" if a == 0 else "pool"
                    P.op(eng, lambda e, a=a, Ct=Ct, v=v: e.tensor_tensor(
                        out=v(ro1), in0=v(qk6), in1=Ct.unsqueeze(1).unsqueeze(1).to_broadcast([128, nheads, 2, 32]),
                        op=ALU.mult), reads=["qk6" + Z, ck], writes=[f"ro1{a}" + Z])
                    P.op("dve", lambda e, St=St, v=v: e.scalar_tensor_tensor(
                        out=v(ro2, 0), in0=v(qk6, 1), scalar=-1.0, in1=St.unsqueeze(1).to_broadcast([128, nheads, 32]),
                        op0=ALU.mult, op1=ALU.mult), reads=["qk6" + Z, sk], writes=[f"ro2{a}0" + Z])
                    P.op(eng, lambda e, St=St, v=v: e.tensor_tensor(
                        out=v(ro2, 1), in0=v(qk6, 0), in1=St.unsqueeze(1).to_broadcast([128, nheads, 32]),
                        op=ALU.mult), reads=["qk6" + Z, sk], writes=[f"ro2{a}1" + Z])
                    P.op(eng, lambda e, v=v: e.tensor_tensor(out=v(qkb), in0=v(ro1), in1=v(ro2), op=ALU.add),
                         reads=[f"ro1{a}" + Z, f"ro2{a}0" + Z, f"ro2{a}1" + Z], writes=["qkb" + Z])

        def qk_c2(q_, dst_stage, dst_key, slot):
            nheads, h0, z = q_
            hs = slice(h0, h0 + nheads)
            qkb = qkbs[z]
            Z = f"_{z}"
            pT = pbb(6 if z == 0 else 5).rearrange("p (k n) -> p k n", k=KD)
            pTk = "pb6" if z == 0 else "pb5"
            for h in range(h0, h0 + nheads):
                P.op("pe", lambda e, h=h: e.transpose(out=pT[:, h, :], in_=qkb[:, h, :], identity=identb[:]),
                     reads=["qkb" + Z, "identb"], writes=[pTk])
            P.op("act", lambda e: e.copy(out=dst_stage[:, hs, slot * 128:(slot + 1) * 128], in_=pT[:, hs, :]),
                 reads=[pTk], writes=[dst_key])


        def qk_fin(q_, rope_tile, dst_stage, dst_key, slot):
            qk_c1(q_, rope_tile)
            qk_c2(q_, dst_stage, dst_key, slot)

        def qk_post(nheads, h0, ps_list, rope_tile, dst_stage, dst_key, slot):
            qk_fin(qk_pre(nheads, h0, ps_list), rope_tile, dst_stage, dst_key, slot)

        def stageB2(hT_src, hkey, slot, own, gi):
            pkv = pbf(0)
            pq = pbf(1)
            for k in range(KD):
                P.op("pe", lambda e, k=k: e.matmul(pkv, lhsT=hT_src[:, k, slot * 128:(slot + 1) * 128],
                                                   rhs=Wi[:, k, 512:1024], start=(k == 0), stop=(k == KD - 1)),
                     reads=[hkey, "Wi1"], writes=["pb0"])
            if own:
                for k in range(KD):
                    P.op("pe", lambda e, k=k: e.matmul(pq, lhsT=hT_src[:, k, slot * 128:(slot + 1) * 128],
                                                       rhs=Wi[:, k, 0:512], start=(k == 0), stop=(k == KD - 1)),
                         reads=[hkey, "Wi0"], writes=["pb1"])
            P.op("act", lambda e: e.copy(out=vst[gi][:, slot, :], in_=pkv[:, 256:512]), reads=["pb0"],
                 writes=[f"vst{gi}"])
            if own:
                return qk_pre(6, 0, [(pq, "pb1", 4, 0), (pkv[:, 0:256], "pb0", 2, 4)])
            return qk_pre(2, 4, [(pkv[:, 0:256], "pb0", 2, 4)])

        def proj_tile(hT_src, hkey, slot, own, rope_tile, gi):
            qk_fin(stageB2(hT_src, hkey, slot, own, gi), rope_tile, kTst[gi], f"kTst{gi}", slot)


        def conv_group(g, hT_src, hkey):
            bi = 2
            for c in range(4):
                ps = pbf(2 + (bi % 3)); pk = f"pb{2 + (bi % 3)}"; bi += 1
                for k in range(KD):
                    P.op("pe", lambda e, k=k, c=c, ps=ps: e.matmul(ps, lhsT=Wi[:, k, 1024 + c * 128:1024 + (c + 1) * 128],
                                                                    rhs=hT_src[:, k, :], start=(k == 0), stop=(k == KD - 1)),
                         reads=[hkey, "Wi2"], writes=[pk])
                P.op("act", lambda e, c=c, ps=ps: e.copy(out=cbT[g % 2][:, c, :], in_=ps), reads=[pk],
                     writes=[f"cbT{g % 2}"])
                ps = pbf(2 + (bi % 3)); pk = f"pb{2 + (bi % 3)}"; bi += 1
                for k in range(KD):
                    P.op("pe", lambda e, k=k, c=c, ps=ps: e.matmul(ps, lhsT=Wi[:, k, 1536 + c * 128:1536 + (c + 1) * 128],
                                                                    rhs=hT_src[:, k, :], start=(k == 0), stop=(k == KD - 1)),
                         reads=[hkey, "Wi3"], writes=[pk])
                P.op("act", lambda e, c=c, ps=ps: e.copy(out=ccs[c % 2], in_=ps), reads=[pk], writes=[f"ccs{c % 2}"])
                ps = pbf(2 + (bi % 3)); pk = f"pb{2 + (bi % 3)}"; bi += 1
                for k in range(KD):
                    P.op("pe", lambda e, k=k, c=c, ps=ps: e.matmul(ps, lhsT=Wi[:, k, 2048 + c * 128:2048 + (c + 1) * 128],
                                                                    rhs=hT_src[:, k, :], start=(k == 0), stop=(k == KD - 1)),
                         reads=[hkey, "Wi4"], writes=[pk])
                P.op("dve", lambda e, c=c, ps=ps: e.tensor_tensor(out=uT[:, c, 1 + g * 512:1 + (g + 1) * 512],
                                                                  in0=ccs[c % 2], in1=ps, op=ALU.mult),
                     reads=[pk, f"ccs{c % 2}"], writes=[f"uT{g}"])

        def conv_final(g):
            lo = 1 + g * 512
            ukeys = [f"uT{g}"] + ([f"uT{g - 1}"] if g > 0 else ["uTh"]) + ([f"uT{g + 1}"] if g < NG - 1 else ["uTh"])
            co = cvo[g % 2]
            WK = ["wcv0", "wcv1", "wcv2"]
            for c in range(4):
                P.op("dve", lambda e, c=c: e.tensor_scalar(out=cvt, in0=uT[:, c, lo - 1:lo + 511], scalar1=wcv[:, 0, c:c + 1],
                                                           scalar2=None, op0=ALU.mult), reads=ukeys + WK, writes=["cvt"])
                P.op("dve", lambda e, c=c: e.scalar_tensor_tensor(out=cvt, in0=uT[:, c, lo:lo + 512], scalar=wcv[:, 1, c:c + 1],
                                                                  in1=cvt, op0=ALU.mult, op1=ALU.add),
                     reads=ukeys + WK + ["cvt"], writes=["cvt"])
                P.op("dve", lambda e, c=c: e.scalar_tensor_tensor(out=cvt, in0=uT[:, c, lo + 1:lo + 513], scalar=wcv[:, 2, c:c + 1],
                                                                  in1=cvt, op0=ALU.mult, op1=ALU.add),
                     reads=ukeys + WK + ["cvt"], writes=["cvt"])
                P.op("pool", lambda e, c=c: e.tensor_tensor(out=co[:, c, :], in0=cvt, in1=cbT[g % 2][:, c, :], op=ALU.mult),
                     reads=["cvt", f"cbT{g % 2}"], writes=[f"cvo{g % 2}"])
            P.dma("pool", lambda e: e.dma_start(out=Cs.rearrange("c d t -> d c t")[:, :, g * 512:(g + 1) * 512], in_=co),
                  reads=[f"cvo{g % 2}"], writes=["Cs"])

        def store_group(gi, tok0, ntile, own_q):
            n = ntile * 128
            P.dma("pool", lambda e: e.dma_start(out=Ks.rearrange("j d s -> d j s")[:, :, tok0:tok0 + n],
                                                in_=kTst[gi][:, 4:6, 0:n]), reads=[f"kTst{gi}"], writes=["Ks"])
            if own_q:
                P.dma("pool", lambda e: e.dma_start(out=Qs.rearrange("h d t -> d h t")[:, :, tok0:tok0 + n],
                                                    in_=kTst[gi][:, 0:4, 0:n]), reads=[f"kTst{gi}"], writes=["Qs"])
            for j in range(2):
                P.dma("pool", lambda e, j=j: e.dma_start(
                    out=Vs[j, tok0:tok0 + n, :].rearrange("(s p) d -> p s d", p=128),
                    in_=vst[gi][:, 0:ntile, j * 128:(j + 1) * 128]), reads=[f"vst{gi}"], writes=["Vs"])

        load_mod(MG, 1, 1, "MG")
        load_mod(MS, 1, 0, "MS")
        gcount = 0
        gi = gcount % 2
        for s_ in range(2):
            norm_tile(ctxb[s_ * 128:(s_ + 1) * 128, :], 128, hTs[gi][:, :, s_ * 128:(s_ + 1) * 128], f"hT{gi}")
            proj_tile(hTs[gi], f"hT{gi}", s_, False, None, gi)
        store_group(gi, S, 2, False)
        gcount += 1
        load_mod(MG, 0, 1, "MG")
        load_mod(MS, 0, 0, "MS")
        gi = gcount % 2
        norm_tile(xh, 2, hTs[gi][:, :, 0:128], f"hT{gi}")
        for c in range(4):
            pcc = pbf(2)
            pcx = pbf(3)
            for k in range(KD):
                P.op("pe", lambda e, k=k, c=c: e.matmul(pcc[:, 0:2], lhsT=Wi[:, k, 1536 + c * 128:1536 + (c + 1) * 128],
                                                        rhs=hTs[gi][:, k, 0:2], start=(k == 0), stop=(k == KD - 1)),
                     reads=[f"hT{gi}", "Wi3"], writes=["pb2"])
            for k in range(KD):
                P.op("pe", lambda e, k=k, c=c: e.matmul(pcx[:, 0:2], lhsT=Wi[:, k, 2048 + c * 128:2048 + (c + 1) * 128],
                                                        rhs=hTs[gi][:, k, 0:2], start=(k == 0), stop=(k == KD - 1)),
                     reads=[f"hT{gi}", "Wi4"], writes=["pb3"])
            P.op("act", lambda e: e.copy(out=ccs[0][:, 0:2], in_=pcc[:, 0:2]), reads=["pb2"], writes=["ccs0"])
            P.op("dve", lambda e: e.tensor_tensor(out=ccs[0][:, 0:2], in0=ccs[0][:, 0:2], in1=pcx[:, 0:2], op=ALU.mult),
                 reads=["pb3", "ccs0"], writes=["ccs0"])
            P.op("dve", lambda e: e.tensor_tensor(out=ccs[0][:, 0:2], in0=ccs[0][:, 0:2], in1=hmt[:, 0:2], op=ALU.mult),
                 reads=["ccs0", "hmt"], writes=["ccs0"])
            P.op("dve", lambda e, c=c: e.tensor_copy(out=uT[:, c, 0:1], in_=ccs[0][:, 0:1]), reads=["ccs0"], writes=["uTh"])
            P.op("dve", lambda e, c=c: e.tensor_copy(out=uT[:, c, TQ + 1:TQ + 2], in_=ccs[0][:, 1:2]),
                 reads=["ccs0", "uTh"], writes=["uTh"])
        gcount += 1
        pend = {}
        for step in range(NT + 4):
            k_ = step - 4
            if 0 <= k_ < NT:
                g, s_ = divmod(k_, 4)
                gi = g % 2
                own = g < NG
                qk_c2(pend[k_][2], kTst[gi], f"kTst{gi}", s_)
                del pend[k_]
                if s_ == 3:
                    store_group(gi, g * 512, 4, own)
                    if own:
                        conv_group(g, hTs[gi], f"hT{gi}")
                        if g >= 1:
                            conv_final(g - 1)
                    if g == NG:
                        conv_final(NG - 1)
            j_ = step - 3
            if 0 <= j_ < NT:
                qk_c1(pend[j_][2], j_)
            j_ = step - 2
            if 0 <= j_ < NT:
                g, s_ = divmod(j_, 4)
                gi = g % 2
                stageB1(pend[j_][1], hTs[gi][:, :, s_ * 128:(s_ + 1) * 128], f"hT{gi}")
                pend[j_][2] = stageB2(hTs[gi], f"hT{gi}", s_, g < NG, gi)
            j_ = step - 1
            if 0 <= j_ < NT:
                pend[j_][1] = stageA2(pend[j_][0])
            if step < NT:
                pend[step] = [stageA1(xr[step * 128:(step + 1) * 128, :], 128), None, None]
        if NT // 4 == NG:
            conv_final(NG - 1)
        P.barrier()
        A.reset(m0)
        if stop <= 2:
            P.emit()
            return nc, P, A

        KT = A.alloc([128, SK], BF16)
        VT = A.alloc([128, NK, 128], BF16)
        qts = [A.alloc([128, 512], BF16) for _ in range(2)]
        pts = [A.alloc([128, 512], BF16) for _ in range(6)]
        rl = A.alloc([128, 512], F32)
        plsb = A.alloc([128, 512], F32)
        onesf = A.alloc([128, 128], F32)
        P.op("pool", lambda e: e.memset(onesf, 1.0), writes=["onesf"])
        obs = [A.alloc([128, 512], BF16) for _ in range(2)]
        gq2 = A.alloc([128, 2, 128], F32)
        negC = A.alloc([128, 4], F32)
        P.dma("sp", lambda e: e.dma_start(out=gq2[:, 0, :], in_=g_q.partition_broadcast(128)), writes=["gq2a"])
        P.dma("sp", lambda e: e.dma_start(out=gq2[:, 1, :], in_=g_k.partition_broadcast(128)), writes=["gq2b"])
        P.op("dve", lambda e: e.tensor_reduce(out=negC[:, 0:2], in_=gq2, axis=AX.X, op=ALU.max, apply_absolute_value=True),
             reads=["gq2a", "gq2b"], writes=["negC"])
        P.op("dve", lambda e: e.tensor_tensor(out=negC[:, 2:3], in0=negC[:, 0:1], in1=negC[:, 1:2], op=ALU.mult),
             reads=["negC"], writes=["negC"])
        P.op("dve", lambda e: e.tensor_scalar(out=negC[:, 3:4], in0=negC[:, 2:3], scalar1=-(128.0 ** 0.5), scalar2=None,
                                              op0=ALU.mult), reads=["negC"], writes=["negC"])
        NCH = TQ // 256
        chunk_i = 0
        for j in range(2):
            nsplit = 4
            cw = SK // nsplit
            for q_ in range(nsplit):
                P.dma("sp", lambda e, q_=q_, j=j: e.dma_start(out=KT[:, q_ * cw:(q_ + 1) * cw],
                                                              in_=Ks[j, :, q_ * cw:(q_ + 1) * cw]),
                      reads=["Ks"], writes=[f"KT{q_}"])
            P.dma("sp", lambda e, j=j: e.dma_start(out=VT, in_=Vs[j].rearrange("(t p) d -> p t d", p=128)),
                  reads=["Vs"], writes=["VT"])
            KTK = [f"KT{q_}" for q_ in range(nsplit)]
            for c in range(NCH):
                qt = qts[chunk_i % 2]; qk_ = f"qt{chunk_i % 2}"
                po = pbf(3 + chunk_i % 2); pok = f"pb{3 + chunk_i % 2}"
                pl = pbf(5 + chunk_i % 2); plk = f"pb{5 + chunk_i % 2}"
                ob = obs[chunk_i % 2]; obk = f"ob{chunk_i % 2}"
                P.dma("sp", lambda e, c=c, j=j, qt=qt: e.dma_start(
                    out=qt.rearrange("p (h t) -> p h t", h=2),
                    in_=Qs.rearrange("h d t -> d h t")[:, 2 * j:2 * j + 2, c * 256:(c + 1) * 256]),
                    reads=["Qs"], writes=[qk_])

                def s_mm(kt):
                    ps = pbf(kt % 3)
                    P.op("pe", lambda e, kt=kt, ps=ps: e.matmul(ps, lhsT=KT[:, kt * 128:(kt + 1) * 128], rhs=qt,
                                                                start=True, stop=True),
                         reads=[qk_] + KTK, writes=[f"pb{kt % 3}"])
                    P.op("act", lambda e, kt=kt, ps=ps: e.activation(out=pts[kt % 6], in_=ps, func=AF.Exp,
                                                                     bias=negC[:, 3:4]),
                         reads=[f"pb{kt % 3}", "negC"], writes=[f"pt{kt % 6}"])

                def pv_mm(kt):
                    P.op("pe", lambda e, kt=kt: e.matmul(po, lhsT=VT[:, kt, :], rhs=pts[kt % 6], start=(kt == 0),
                                                         stop=(kt == NK - 1)), reads=[f"pt{kt % 6}", "VT"], writes=[pok])

                NGL = (NK + 2) // 3
                RLAST = NK - 3 * (NGL - 1)

                def l_group(gl):
                    for jj in range(3):
                        kt = gl * 3 + jj
                        if kt >= NK:
                            break
                        last = (gl == NGL - 1) if jj < RLAST else (gl == NGL - 2)
                        P.op("pe", lambda e, kt=kt, jj=jj, last=last: e.matmul(
                            pl[32 * jj:32 * jj + 32, :], lhsT=onesb[:, 0:32], rhs=pts[kt % 6], start=(gl == 0), stop=last,
                            tile_position=(0, 32 * jj)), reads=[f"pt{kt % 6}", "onesb"], writes=[plk])
                s_mm(0)
                s_mm(1)
                for kt in range(NK):
                    if kt + 2 < NK:
                        s_mm(kt + 2)
                    pv_mm(kt)
                    if kt % 3 == 2 or kt == NK - 1:
                        l_group(kt // 3)
                P.op("act", lambda e: e.copy(out=plsb[0:96, :], in_=pl[0:96, :]), reads=[plk], writes=["plsb"])
                P.op("pe", lambda e: e.matmul(pbf(7), lhsT=onesf[0:96, :], rhs=plsb[0:96, :], start=True, stop=True),
                     reads=["plsb", "onesf"], writes=["pb7"])
                P.op("dve", lambda e: e.reciprocal(out=rl, in_=pbf(7)), reads=["pb7"], writes=["rl"])
                P.op("dve", lambda e: e.scalar_tensor_tensor(out=ob, in0=po, scalar=32.0, in1=rl, op0=ALU.mult, op1=ALU.mult),
                     reads=[pok, "rl"], writes=[obk])
                P.dma("pool", lambda e, c=c, j=j, ob=ob: e.dma_start(
                    out=As.rearrange("h d t -> d h t")[:, 2 * j:2 * j + 2, c * 256:(c + 1) * 256],
                    in_=ob.rearrange("p (h t) -> p h t", h=2)), reads=[obk], writes=["As"])
                chunk_i += 1
        P.barrier()
        A.reset(m0)
        if stop <= 3:
            P.emit()
            return nc, P, A

        Wo = A.alloc([128, KD, D], BF16)
        GT1 = A.alloc([128, D], F32)
        G2 = A.alloc([128, D], F32)
        SH2 = A.alloc([128, D], F32)
        GT2 = A.alloc([128, D], F32)
        Wr = A.alloc([128, KD, NE], F32)
        BR = A.alloc([128, NE], F32)
        BD = A.alloc([NE, D], F32)
        EOFF = A.alloc([128, NE], F32)
        cnt = A.alloc([128, NE], F32)
        DEST = A.alloc([128, NQ, 4], I32)
        GKT = A.alloc([128, NQ, 4], F32)
        mixT = [A.alloc([128, KD, 512], BF16) for _ in range(2)]
        NPA = 8
        xt4 = [A.alloc([128, D], F32) for _ in range(3)]
        x1t_l = [A.alloc([128, D], F32) for _ in range(NPA)]
        h2bs = [A.alloc([128, D], BF16) for _ in range(NPA)]
        x1p = [A.alloc([128, D], F32) for _ in range(2)]
        h2f_l = [A.alloc([128, D], F32) for _ in range(2)]
        h2T_l = [A.alloc([128, KD, 128], F32) for _ in range(2)]
        junk4 = A.alloc([128, D], BF16)
        tmp4_l = [A.alloc([128, D], F32) for _ in range(2)]
        gmT_l = [A.alloc([NE, 128], F32) for _ in range(2)]
        sm_l = [A.alloc([128, 8], F32) for _ in range(2)]
        r4_l = [A.alloc([128, 12, 4 * NE], F32) for _ in range(2)]
        maskb4_l = [A.alloc([128, 4 * NE], BF16) for _ in range(2)]
        m84_l = [A.alloc([128, 4, 8], F32) for _ in range(2)]
        t84_l = [A.alloc([128, 4, 8], F32) for _ in range(2)]
        sm4_l = [A.alloc([128, 8, 4], F32) for _ in range(2)]

        P.dma("pool", lambda e: e.dma_start(out=Wo, in_=w_out.rearrange("(k p) n -> p k n", p=128)), writes=["Wo"])
        load_mod(GT1, 0, 2, "GT1")
        load_mod(G2, 0, 4, "G2")
        load_mod(SH2, 0, 3, "SH2")
        load_mod(GT2, 0, 5, "GT2")
        P.dma("sp", lambda e: e.dma_start(out=Wr, in_=w_router.rearrange("(k p) n -> p k n", p=128)), writes=["Wr"])
        P.dma("sp", lambda e: e.dma_start(out=BR, in_=b_router.partition_broadcast(128)), writes=["BR"])
        P.dma("sp", lambda e: e.dma_start(out=BD, in_=b_down), writes=["BD"])
        P.op("pool", lambda e: e.iota(EOFF, pattern=[[C, NE]], base=0, channel_multiplier=0,
                                      allow_small_or_imprecise_dtypes=True), writes=["EOFF"])
        P.op("dve", lambda e: e.memset(cnt, 0.0), writes=["cnt"])

        def v4(ap):
            return ap.rearrange("p (t e) -> p t e", t=4)

        def bc4(ap):
            return ap.to_broadcast([128, 4, NE])

        def p4A(i):
            g, s_ = divmod(i, 4)
            mx = mixT[g % 2]; mk_ = f"mixT{g % 2}"
            z2, z8, gz = i % 2, i % NPA, g % 2
            xt = xt4[i % 3]; xk = f"x4{i % 3}"
            x1t, h2b, h2f, tmp4, h2T, sm = x1t_l[z8], h2bs[z8], h2f_l[z2], tmp4_l[z2], h2T_l[z2], sm_l[z2]
            ss4, rs4 = sm[:, 0:1], sm[:, 1:2]
            ZA = f"@{z2}"
            if s_ == 0:
                P.dma("sp", lambda e: e.dma_start(out=mx[:, 0:4, :], in_=As.rearrange("h d t -> d h t")[:, :, g * 512:(g + 1) * 512]),
                      reads=["As"], writes=[mk_ + "a"])
                P.dma("sp", lambda e: e.dma_start(out=mx[:, 4:8, :], in_=Cs.rearrange("c d t -> d c t")[:, :, g * 512:(g + 1) * 512]),
                      reads=["Cs"], writes=[mk_ + "c"])
            P.dma("sp", lambda e: e.dma_start(out=xt, in_=xr[i * 128:(i + 1) * 128, :]), writes=[xk])
            for half in range(2):
                ps = pbf(half); pk = f"pb{half}"
                for k in range(KD):
                    P.op("pe", lambda e: e.matmul(ps, lhsT=mx[:, k, s_ * 128:(s_ + 1) * 128], rhs=Wo[:, k, half * 512:(half + 1) * 512],
                                                  start=(k == 0), stop=(k == KD - 1)), reads=[mk_ + "a", mk_ + "c", "Wo"], writes=[pk])
                hs_ = slice(half * 512, (half + 1) * 512)
                P.op("dve", lambda e: e.tensor_tensor(out=x1t[:, hs_], in0=ps, in1=GT1[:, hs_], op=ALU.mult),
                     reads=[pk, "GT1"], writes=[f"x1t{half}@{z8}"])
                P.op("dve", lambda e: e.tensor_tensor(out=x1t[:, hs_], in0=x1t[:, hs_], in1=xt[:, hs_], op=ALU.add),
                     reads=[f"x1t{half}@{z8}", xk], writes=[f"x1t{half}@{z8}"])
            X1K = [f"x1t0@{z8}", f"x1t1@{z8}"]
            P.op("act", lambda e: e.activation(out=junk4, in_=x1t, func=AF.Square, accum_out=ss4), reads=X1K, writes=["junk4", "ss4" + ZA])
            P.op("act", lambda e: e.activation(out=rs4, in_=ss4, func=AF.Sqrt, scale=1.0 / D, bias=epsb[:, 0:1]),
                 reads=["ss4" + ZA, "epsb"], writes=["rs4" + ZA])
            P.op("dve", lambda e: e.reciprocal(out=rs4, in_=rs4), reads=["rs4" + ZA], writes=["rs4" + ZA])
            P.op("dve", lambda e: e.scalar_tensor_tensor(out=tmp4, in0=x1t, scalar=rs4, in1=G2, op0=ALU.mult, op1=ALU.mult),
                 reads=X1K + ["rs4" + ZA, "G2"], writes=["tmp4" + ZA])
            P.op("pool", lambda e: e.tensor_tensor(out=h2f, in0=tmp4, in1=SH2, op=ALU.add), reads=["tmp4" + ZA, "SH2"], writes=["h2f" + ZA])
            P.op("act", lambda e: e.copy(out=h2b, in_=h2f), reads=["h2f" + ZA], writes=[f"h2b@{z8}"])
            if dbg:
                P.dma("sp", lambda e: e.dma_start(out=H2[i * 128:(i + 1) * 128, :], in_=h2f), reads=["h2f" + ZA])

        def p4A2(i):
            g, s_ = divmod(i, 4)
            z2, gz = i % 2, g % 2
            h2f, h2T = h2f_l[z2], h2T_l[z2]
            ZA = f"@{z2}"
            for hf in range(2):
                pt_ = pbf(2 + hf).rearrange("p (k n) -> p k n", k=4)
                for k4 in range(4):
                    k = hf * 4 + k4
                    P.op("pe", lambda e: e.transpose(out=pt_[:, k4, :], in_=h2f[:, k * 128:(k + 1) * 128], identity=identf[:]),
                         reads=["h2f" + ZA, "identf"], writes=[f"pb{2 + hf}"])
                P.op("act" if hf == 0 else "dve", lambda e: (e.copy if hf == 0 else e.tensor_copy)(
                    out=h2T[:, hf * 4:(hf + 1) * 4, :], in_=pt_), reads=[f"pb{2 + hf}"], writes=[f"h2T{hf}" + ZA])
            plg = pbf(4)
            for k in range(KD):
                P.op("pe", lambda e: e.matmul(plg[:, 0:NE], lhsT=h2T[:, k, :], rhs=Wr[:, k, :], start=(k == 0), stop=(k == KD - 1)),
                     reads=["h2T0" + ZA, "h2T1" + ZA, "Wr"], writes=["pb4"])
            lg4 = v4(r4_l[gz][:, 0, :])
            P.op("dve", lambda e: e.tensor_tensor(out=lg4[:, s_, :], in0=plg[:, 0:NE], in1=BR, op=ALU.add),
                 reads=["pb4", "BR"], writes=[f"lg4@{gz}"])
            if dbg:
                P.dma("sp", lambda e: e.dma_start(out=LG[i * 128:(i + 1) * 128, :], in_=lg4[:, s_, :]), reads=[f"lg4@{gz}"])

        def p4B(g):
            gz = g % 2
            G = f"@{gz}"
            r4 = r4_l[gz]
            lg4, mask4, ex4, gm4, pos4, val4, dstf4, nd4, eq4, gv4 = (v4(r4[:, i_, :]) for i_ in range(10))
            maskb4 = maskb4_l[gz]
            m84, t84, sm4 = m84_l[gz], t84_l[gz], sm4_l[gz]
            den4 = sm4[:, 0, :]
            dk4 = sm4[:, 4:8, :].rearrange("p k t -> p t k")
            for t in range(4):
                P.op("dve", lambda e: e.max(out=m84[:, t, :], in_=lg4[:, t, :]), reads=["lg4" + G], writes=["m84" + G])
            P.op("dve", lambda e: e.tensor_tensor(out=mask4, in0=lg4, in1=bc4(m84[:, :, 3:4]), op=ALU.is_ge),
                 reads=["lg4" + G, "m84" + G], writes=["mask4" + G])
            P.op("dve", lambda e: e.tensor_tensor(out=ex4, in0=lg4, in1=bc4(m84[:, :, 0:1]), op=ALU.subtract),
                 reads=["lg4" + G, "m84" + G], writes=["ex4" + G])
            P.op("act", lambda e: e.activation(out=ex4, in_=ex4, func=AF.Exp), reads=["ex4" + G], writes=["ex4" + G])
            P.op("dve", lambda e: e.tensor_tensor(out=ex4, in0=ex4, in1=mask4, op=ALU.mult), reads=["ex4" + G, "mask4" + G], writes=["ex4" + G])
            P.op("dve", lambda e: e.tensor_reduce(out=den4, in_=ex4, axis=AX.X, op=ALU.add), reads=["ex4" + G], writes=["den4" + G])
            P.op("dve", lambda e: e.reciprocal(out=den4, in_=den4), reads=["den4" + G], writes=["den4" + G])
            P.op("dve", lambda e: e.tensor_tensor(out=gm4, in0=ex4, in1=bc4(den4.unsqueeze(2)), op=ALU.mult),
                 reads=["ex4" + G, "den4" + G], writes=["gm4" + G])
            P.op("dve", lambda e: e.tensor_copy(out=maskb4, in_=r4[:, 1, :]), reads=["mask4" + G], writes=["maskb4" + G])
            ppp = pbf(5)
            for t in range(4):
                for t2 in range(t + 1):
                    P.op("pe", lambda e: e.matmul(ppp[:, t * NE:(t + 1) * NE], lhsT=(utri[:] if t2 == t else onesb[:]),
                                                  rhs=maskb4[:, t2 * NE:(t2 + 1) * NE], start=(t2 == 0), stop=(t2 == t)),
                         reads=["utri", "onesb", "maskb4" + G], writes=["pb5"])
            for t2 in range(4):
                P.op("pe", lambda e: e.matmul(ppp[:, 4 * NE:5 * NE], lhsT=onesb[:], rhs=maskb4[:, t2 * NE:(t2 + 1) * NE],
                                              start=(t2 == 0), stop=(t2 == 3)), reads=["onesb", "maskb4" + G], writes=["pb5"])
            P.op("dve", lambda e: e.tensor_tensor(out=pos4, in0=v4(ppp[:, 0:4 * NE]), in1=cnt.unsqueeze(1).to_broadcast([128, 4, NE]), op=ALU.add),
                 reads=["pb5", "cnt"], writes=["pos4" + G])
            P.op("dve", lambda e: e.tensor_tensor(out=cnt, in0=ppp[:, 4 * NE:5 * NE], in1=cnt, op=ALU.add),
                 reads=["pb5", "cnt", "pos4" + G], writes=["cnt"])
            P.op("dve", lambda e: e.scalar_tensor_tensor(out=val4, in0=pos4, scalar=float(C), in1=mask4, op0=ALU.is_lt, op1=ALU.mult),
                 reads=["pos4" + G, "mask4" + G], writes=["val4" + G])
            P.op("dve", lambda e: e.tensor_tensor(out=dstf4, in0=pos4, in1=EOFF.unsqueeze(1).to_broadcast([128, 4, NE]), op=ALU.add),
                 reads=["pos4" + G, "EOFF"], writes=["dstf4" + G])
            P.op("dve", lambda e: e.tensor_scalar(out=nd4, in0=dstf4, scalar1=-1.0, scalar2=BIG, op0=ALU.mult, op1=ALU.add),
                 reads=["dstf4" + G], writes=["nd4" + G])
            P.op("dve", lambda e: e.tensor_tensor(out=nd4, in0=nd4, in1=val4, op=ALU.mult), reads=["nd4" + G, "val4" + G], writes=["nd4" + G])
            P.op("dve", lambda e: e.tensor_scalar(out=nd4, in0=nd4, scalar1=-BIG, scalar2=None, op0=ALU.add), reads=["nd4" + G], writes=["nd4" + G])
            for t in range(4):
                P.op("dve", lambda e: e.max(out=t84[:, t, :], in_=nd4[:, t, :]), reads=["nd4" + G], writes=["t84" + G])
            P.op("dve", lambda e: e.tensor_scalar(out=dk4, in0=t84[:, :, 0:4], scalar1=-1.0, scalar2=None, op0=ALU.mult),
                 reads=["t84" + G], writes=["dk4" + G])
            P.op("dve", lambda e: e.tensor_copy(out=DEST[:, 4 * g:4 * g + 4, :], in_=dk4), reads=["dk4" + G],
                 writes=[f"DEST{4 * g + t}" for t in range(4)])
            P.op("dve", lambda e: e.tensor_tensor(out=gv4, in0=gm4, in1=val4, op=ALU.mult), reads=["gm4" + G, "val4" + G], writes=["gv4" + G])
            for k in range(4):
                P.op("dve", lambda e: e.tensor_tensor(out=eq4, in0=nd4, in1=bc4(t84[:, :, k:k + 1]), op=ALU.is_equal),
                     reads=["nd4" + G, "t84" + G], writes=["eq4" + G])
                P.op("dve", lambda e: e.tensor_tensor(out=eq4, in0=eq4, in1=gv4, op=ALU.mult), reads=["eq4" + G, "gv4" + G], writes=["eq4" + G])
                P.op("dve", lambda e: e.tensor_reduce(out=GKT[:, 4 * g:4 * g + 4, k], in_=eq4, axis=AX.X, op=ALU.add),
                     reads=["eq4" + G], writes=[f"GKT{4 * g + t}" for t in range(4)])
            if dbg:
                for t in range(4):
                    i = 4 * g + t
                    P.dma("sp", lambda e: e.dma_start(out=DST[i * 128:(i + 1) * 128, :], in_=DEST[:, i, :]), reads=[f"DEST{i}"])
                    P.dma("sp", lambda e: e.dma_start(out=GKD[i * 128:(i + 1) * 128, :], in_=GKT[:, i, :]), reads=[f"GKT{i}"])

        def p4C(i):
            g, s_ = divmod(i, 4)
            z2, z8, gz = i % 2, i % NPA, g % 2
            x1t, h2b, xp, gmT = x1t_l[z8], h2bs[z8], x1p[z2], gmT_l[z2]
            xpk = f"x1p{z2}"
            gm = v4(r4_l[gz][:, 3, :])[:, s_, :]
            pgt = pbf(6)
            P.op("pe", lambda e: e.transpose(out=pgt[0:NE, 0:128], in_=gm, identity=identf[:]),
                 reads=[f"gm4@{gz}", "identf"], writes=["pb6"])
            P.op("act", lambda e: e.copy(out=gmT, in_=pgt[0:NE, 0:128]), reads=["pb6"], writes=[f"gmT@{z2}"])
            for half in range(2):
                ps = pbf(7); pk = "pb7"
                hs_ = slice(half * 512, (half + 1) * 512)
                P.op("pe", lambda e: e.matmul(ps, lhsT=gmT, rhs=BD[:, hs_], start=True, stop=True), reads=[f"gmT@{z2}", "BD"], writes=[pk])
                P.op("dve", lambda e: e.tensor_tensor(out=xp[:, hs_], in0=ps, in1=GT2[:, hs_], op=ALU.mult),
                     reads=[pk, "GT2"], writes=[xpk + str(half)])
                P.op("pool", lambda e: e.tensor_tensor(out=xp[:, hs_], in0=xp[:, hs_], in1=x1t[:, hs_], op=ALU.add),
                     reads=[xpk + str(half), f"x1t{half}@{z8}"], writes=[xpk + str(half)])
            P.dma("sp", lambda e: e.dma_start(out=X1[i * 128:(i + 1) * 128, :], in_=xp), reads=[xpk + "0", xpk + "1"], writes=["X1"])
            for k in range(4):
                P.dma("pool", lambda e: e.indirect_dma_start(
                    out=XB, out_offset=bass.IndirectOffsetOnAxis(ap=DEST[:, i, k:k + 1], axis=0),
                    in_=h2b, in_offset=None, bounds_check=BCREG, oob_is_err=False),
                    reads=[f"h2b@{z8}", f"DEST{i}"], writes=["XB"])

        for g in range(NG + 1):
            for s_ in range(4):
                if g < NG:
                    p4A(4 * g + s_)
                    if s_ >= 1:
                        p4A2(4 * g + s_ - 1)
                if g >= 1:
                    p4C(4 * (g - 1) + s_)
            if g < NG:
                p4A2(4 * g + 3)
                p4B(g)
        DEST_KEYS = [f"DEST{i}" for i in range(NQ)]
        GKT_KEYS = [f"GKT{i}" for i in range(NQ)]
        if stop <= 4:
            P.barrier()
            P.emit()
            return nc, P, A

        DESTp = sb("DESTp", [128, NQ, 4], I32)
        GKTp = sb("GKTp", [128, NQ, 4], F32)
        P.op("dve", lambda e: e.tensor_copy(out=DESTp[:], in_=DEST), reads=DEST_KEYS, writes=["DESTp"])
        P.op("dve", lambda e: e.tensor_copy(out=GKTp[:], in_=GKT), reads=GKT_KEYS, writes=["GKTp"])
        P.barrier()
        A.reset(m0)

        Wgu = [A.alloc([128, KD, 2 * D], BF16) for _ in range(2)]
        Wd = [A.alloc([128, KD, D], BF16) for _ in range(2)]
        xbT = A.alloc([128, KD, C], BF16)
        aT = A.alloc([128, KD, C], BF16)
        xs2 = [A.alloc([128, NSB, D], BF16) for _ in range(2)]
        bguT = A.alloc([128, 16, NE], F32)
        bgl = A.alloc([NE, 2 * D], F32)
        gsb = [A.alloc([128, SN], F32) for _ in range(2)]
        sgb = [A.alloc([128, SN], F32) for _ in range(2)]
        lsb = [A.alloc([128, SN], F32) for _ in range(2)]
        yst = [A.alloc([128, D], F32) for _ in range(5)]
        P.dma("sp", lambda e: e.dma_start(out=bgl, in_=b_gate_up), writes=["bgl"])
        for m in range(16):
            pt_ = pbf(m % 2)
            P.op("pe", lambda e, m=m, pt_=pt_: e.transpose(out=pt_[:, 0:NE], in_=bgl[:, m * 128:(m + 1) * 128],
                                                           identity=identf[0:NE, 0:NE]),
                 reads=["bgl", "identf"], writes=[f"pb{m % 2}"])
            P.op("act", lambda e, m=m, pt_=pt_: e.copy(out=bguT[:, m, :], in_=pt_[:, 0:NE]), reads=[f"pb{m % 2}"],
                 writes=["bguT"])

        def load_w(ei):
            b = ei % 2
            for q_ in range(4):
                P.dma("pool", lambda e, q_=q_: e.dma_start(
                    out=Wgu[b][:, :, q_ * 512:(q_ + 1) * 512],
                    in_=w_gate_up[ei, :, q_ * 512:(q_ + 1) * 512].rearrange("(k p) n -> p k n", p=128)),
                    writes=[f"Wgu{b}_{q_}"])
            for q_ in range(2):
                P.dma("pool", lambda e, q_=q_: e.dma_start(
                    out=Wd[b][:, :, q_ * 512:(q_ + 1) * 512],
                    in_=w_down[ei, :, q_ * 512:(q_ + 1) * 512].rearrange("(k p) n -> p k n", p=128)),
                    writes=[f"Wd{b}_{q_}"])

        def load_x(ei):
            P.dma("sp", lambda e: e.dma_start(out=xs2[ei % 2],
                                              in_=XB[ei * C:(ei + 1) * C, :].rearrange("(s p) d -> p s d", p=128)),
                  reads=["XB"], writes=[f"xs2_{ei % 2}"])

        load_w(0)
        load_x(0)
        ev_i = 0
        for ei in range(NE):
            b = ei % 2
            if ei + 1 < NE:
                load_w(ei + 1)
                load_x(ei + 1)
            for sbk in range(NSB):
                xs = xs2[ei % 2][:, sbk, :]; xsk = f"xs2_{ei % 2}"
                pT = pbb(6 + sbk % 2).rearrange("p (k n) -> p k n", k=KD)
                for k in range(KD):
                    P.op("pe", lambda e, k=k, xs=xs, pT=pT: e.transpose(out=pT[:, k, :], in_=xs[:, k * 128:(k + 1) * 128],
                                                                        identity=identb[:]),
                         reads=[xsk, "identb"], writes=[f"pb{6 + sbk % 2}"])
                P.op("act", lambda e, sbk=sbk, pT=pT: e.copy(out=xbT[:, :, sbk * 128:(sbk + 1) * 128], in_=pT),
                     reads=[f"pb{6 + sbk % 2}"], writes=[f"xbT{sbk // (SN // 128)}"])
            for n in range(C // SN):
                ns = slice(n * SN, (n + 1) * SN)
                for m in range(8):
                    pg = pbf(0 + (m % 2) * 2); pgk = f"pb{(m % 2) * 2}"
                    plin = pbf(1 + (m % 2) * 2); plk_ = f"pb{1 + (m % 2) * 2}"
                    for k in range(KD):
                        P.op("pe", lambda e, k=k, m=m, pg=pg: e.matmul(pg[:, 0:SN], lhsT=Wgu[b][:, k, m * 128:(m + 1) * 128],
                                                                        rhs=xbT[:, k, ns], start=(k == 0), stop=(k == KD - 1)),
                             reads=[f"Wgu{b}_{m // 4}", f"xbT{n}"], writes=[pgk])
                    for k in range(KD):
                        P.op("pe", lambda e, k=k, m=m, plin=plin: e.matmul(plin[:, 0:SN], lhsT=Wgu[b][:, k, D + m * 128:D + (m + 1) * 128],
                                                                           rhs=xbT[:, k, ns], start=(k == 0), stop=(k == KD - 1)),
                             reads=[f"Wgu{b}_{2 + m // 4}", f"xbT{n}"], writes=[plk_])
                    gs = gsb[ev_i % 2]; sg = sgb[ev_i % 2]; ls = lsb[ev_i % 2]; evk = str(ev_i % 2); ev_i += 1
                    P.op("dve", lambda e, m=m, pg=pg, gs=gs: e.tensor_scalar(out=gs, in0=pg[:, 0:SN], scalar1=bguT[:, m, ei:ei + 1],
                                                                             scalar2=7.0, op0=ALU.add, op1=ALU.min),
                         reads=[pgk, "bguT"], writes=["gs" + evk])
                    P.op("act", lambda e, gs=gs, sg=sg: e.activation(out=sg, in_=gs, func=AF.Sigmoid, scale=1.702),
                         reads=["gs" + evk], writes=["sg" + evk])
                    P.op("dve", lambda e, m=m, plin=plin, ls=ls: e.tensor_scalar(out=ls, in0=plin[:, 0:SN],
                                                                                scalar1=bguT[:, 8 + m, ei:ei + 1], scalar2=7.0,
                                                                                op0=ALU.add, op1=ALU.min),
                         reads=[plk_, "bguT"], writes=["ls" + evk])
                    P.op("dve", lambda e, ls=ls: e.tensor_scalar(out=ls, in0=ls, scalar1=-7.0, scalar2=1.0, op0=ALU.max, op1=ALU.add),
                         reads=["ls" + evk], writes=["ls" + evk])
                    P.op("pool", lambda e, gs=gs, sg=sg: e.tensor_tensor(out=gs, in0=gs, in1=sg, op=ALU.mult),
                         reads=["gs" + evk, "sg" + evk], writes=["gs" + evk])
                    P.op("dve", lambda e, m=m, gs=gs, ls=ls: e.tensor_tensor(out=aT[:, m, ns], in0=gs, in1=ls, op=ALU.mult),
                         reads=["gs" + evk, "ls" + evk], writes=[f"aT{n}"])
            for sbk in range(NSB):
                ys = yst[sbk % 5]; ysk = f"yst{sbk % 5}"
                for half in range(2):
                    bk_ = (4, 5, 0, 1, 2, 3)[(sbk * 2 + half) % 6]
                    ps = pbf(bk_); pk = f"pb{bk_}"
                    for k in range(KD):
                        P.op("pe", lambda e, k=k, half=half, sbk=sbk, ps=ps: e.matmul(
                            ps, lhsT=aT[:, k, sbk * 128:(sbk + 1) * 128], rhs=Wd[b][:, k, half * 512:(half + 1) * 512],
                            start=(k == 0), stop=(k == KD - 1)), reads=[f"aT{sbk // (SN // 128)}", f"Wd{b}_{half}"], writes=[pk])
                    P.op("act" if half == 0 else "dve", lambda e, half=half, ps=ps, ys=ys: (e.copy if half == 0 else e.tensor_copy)(
                        out=ys[:, half * 512:(half + 1) * 512], in_=ps), reads=[pk], writes=[ysk + str(half)])
                P.dma("sp", lambda e, sbk=sbk, ys=ys: e.dma_start(out=Y[ei * C + sbk * 128:ei * C + (sbk + 1) * 128, :], in_=ys),
                      reads=[ysk + "0", ysk + "1"], writes=["Y"])
        P.barrier()
        A.reset(m0)
        if stop <= 5:
            P.emit()
            return nc, P, A

        GT2b = A.alloc([128, D], F32)
        GF = A.alloc([128, D], F32)
        ygs2 = [[A.alloc([128, D], F32) for _ in range(4)] for _ in range(3)]
        x1s = [A.alloc([128, D], F32) for _ in range(3)]
        accs = [A.alloc([128, D], F32) for _ in range(2)]
        outs = [A.alloc([128, D], F32) for _ in range(2)]
        junk6 = A.alloc([128, D], BF16)
        sm6 = A.alloc([128, 8], F32)
        load_mod(GT2b, 0, 5, "GT2b")
        P.dma("sp", lambda e: e.dma_start(out=GF, in_=g_final.partition_broadcast(128)), writes=["GF"])
        for z_ in range(3):
            for q_ in range(4):
                P.op("pool", lambda e: e.memset(ygs2[z_][q_], 0.0), writes=[f"yg{q_}_{z_}"])

        def a6(i):
            z_ = i % 2
            y_ = i % 3
            P.dma("sp", lambda e: e.dma_start(out=x1s[y_], in_=X1[i * 128:(i + 1) * 128, :]), reads=["X1"], writes=[f"x1s{y_}"])
            for k in range(4):
                P.dma("pool", lambda e: e.indirect_dma_start(
                    out=ygs2[y_][k], out_offset=None, in_=Y,
                    in_offset=bass.IndirectOffsetOnAxis(ap=DESTp[:, i, k:k + 1], axis=0),
                    bounds_check=BCREG, oob_is_err=False), reads=["Y", "DESTp"], writes=[f"yg{k}_{y_}"])

        def b6a(i):
            z_ = i % 2
            y_ = i % 3
            ygs = ygs2[y_]
            x1 = x1s[y_]; x1k = f"x1s{y_}"
            acc = accs[z_]; ak = f"acc{z_}"
            ot = outs[z_]; ok = f"out{z_}"
            P.op("dve", lambda e: e.tensor_scalar(out=acc, in0=ygs[0], scalar1=GKTp[:, i, 0:1], scalar2=None, op0=ALU.mult),
                 reads=[f"yg0_{y_}", "GKTp"], writes=[ak])
            for k in range(1, 4):
                P.op("dve", lambda e: e.scalar_tensor_tensor(out=acc, in0=ygs[k], scalar=GKTp[:, i, k:k + 1], in1=acc,
                                                             op0=ALU.mult, op1=ALU.add),
                     reads=[f"yg{k}_{y_}", "GKTp", ak], writes=[ak])
            P.op("pool", lambda e: e.tensor_tensor(out=acc, in0=acc, in1=GT2b, op=ALU.mult), reads=[ak, "GT2b"], writes=[ak])
            P.op("dve", lambda e: e.tensor_tensor(out=acc, in0=acc, in1=x1, op=ALU.add), reads=[ak, x1k], writes=[ak])

        def b6b(i):
            z_ = i % 2
            acc = accs[z_]; ak = f"acc{z_}"
            ot = outs[z_]; ok = f"out{z_}"
            P.op("act", lambda e: e.activation(out=junk6, in_=acc, func=AF.Square, accum_out=sm6[:, 2 * z_:2 * z_ + 1]),
                 reads=[ak], writes=["junk6", f"ss_f{z_}"])
            P.op("act", lambda e: e.activation(out=sm6[:, 2 * z_ + 1:2 * z_ + 2], in_=sm6[:, 2 * z_:2 * z_ + 1], func=AF.Sqrt,
                                               scale=1.0 / D, bias=epsb[:, 0:1]), reads=[f"ss_f{z_}", "epsb"], writes=[f"rs_f{z_}"])
            P.op("dve", lambda e: e.reciprocal(out=sm6[:, 2 * z_ + 1:2 * z_ + 2], in_=sm6[:, 2 * z_ + 1:2 * z_ + 2]),
                 reads=[f"rs_f{z_}"], writes=[f"rs_f{z_}"])
            P.op("dve", lambda e: e.scalar_tensor_tensor(out=ot, in0=acc, scalar=sm6[:, 2 * z_ + 1:2 * z_ + 2], in1=GF,
                                                         op0=ALU.mult, op1=ALU.mult), reads=[ak, f"rs_f{z_}", "GF"], writes=[ok])
            P.dma("sp", lambda e: e.dma_start(out=yout[i * 128:(i + 1) * 128, :], in_=ot), reads=[ok], writes=["yout"])

        def a6x(i):
            pass

        for step in range(NQ + 3):
            if step < NQ:
                a6(step)
            if 0 <= step - 2 < NQ:
                b6a(step - 2)
            if 0 <= step - 3 < NQ:
                b6b(step - 3)
        P.emit()
    return nc, P, A


_CACHE = {}


def make_in_maps(S, inputs, moe_in=True):
    x = np.ascontiguousarray(inputs["x"], dtype=np.float32)
    TQ = S // 4
    maps = []
    for core in range(8):
        b = core // 4
        t0 = (core % 4) * TQ
        xr = np.ascontiguousarray(np.roll(x[b], -t0, axis=0))
        xh = np.zeros((2, D), np.float32)
        if t0 > 0:
            xh[0] = x[b, t0 - 1]
        if t0 + TQ < S:
            xh[1] = x[b, t0 + TQ]
        hm = np.zeros((128, 2), np.float32)
        hm[:, 0] = 1.0 if t0 > 0 else 0.0
        hm[:, 1] = 1.0 if t0 + TQ < S else 0.0
        m = {
            "xr": xr, "xh": xh, "hm": hm,
            "r0": np.full((128, 1), t0 // 64, np.float32),
            "cvec": np.ascontiguousarray(np.stack([inputs["c"][b], inputs["c_ctx"]]).astype(np.float32)),
            "ctxb": np.ascontiguousarray(inputs["ctx"][b], dtype=np.float32),
            "w_ada": np.ascontiguousarray(inputs["w_ada"][0]),
            "b_ada": np.ascontiguousarray(inputs["b_ada"][0]).reshape(1, -1),
            "g_norm1": np.ascontiguousarray(inputs["g_norm1"][0]),
            "g_norm2": np.ascontiguousarray(inputs["g_norm2"][0]),
            "w_in": np.ascontiguousarray(inputs["w_in"][0]),
            "g_q": np.ascontiguousarray(inputs["g_q"][0]),
            "g_k": np.ascontiguousarray(inputs["g_k"][0]),
            "w_conv": np.ascontiguousarray(inputs["w_conv"][0]),
            "w_out": np.ascontiguousarray(inputs["w_out"][0]),
            "w_router": np.ascontiguousarray(inputs["w_router"][0]),
            "b_router": np.ascontiguousarray(inputs["b_router"][0]),
            "w_gate_up": np.ascontiguousarray(inputs["w_gate_up"][0]) if moe_in else np.zeros((1, 1, 2), np.float32),
            "b_gate_up": np.ascontiguousarray(inputs["b_gate_up"][0]),
            "w_down": np.ascontiguousarray(inputs["w_down"][0]) if moe_in else np.zeros((1, 1, 2), np.float32),
            "b_down": np.ascontiguousarray(inputs["b_down"][0]),
            "g_final": np.ascontiguousarray(inputs["g_final"]),
        }
        maps.append({k: np.asarray(v) for k, v in m.items()})
    return maps


def kernel(**inputs):
    inputs = {k: np.asarray(v) for k, v in inputs.items()}
    B, S, _ = inputs["x"].shape
    assert B == 2
    C = max(128, (S // 4) * 4 // NE * 2)
    key = (S, C)
    if key not in _CACHE:
        _CACHE[key] = build(S, C)[0]
    nc = _CACHE[key]
    maps = make_in_maps(S, inputs)
    res = run_bass_kernel_spmd(nc, maps, core_ids=list(range(8)))
    TQ = S // 4
    out = np.empty((B, S, D), np.float32)
    for core in range(8):
        b = core // 4
        t0 = (core % 4) * TQ
        out[b, t0:t0 + TQ] = res.results[core]["yout"]
    return out
```
